# Optimizing a Trainium2 kernel written in Bass

```python
import jax, jax.numpy as jnp
from jax import lax
import numpy as np

D_MODEL = 1024
BATCH = 2
SEQ = 16384
DEPTH = 2

N_HEADS = 8
Q_LORA = 256
KV_LORA = 128
QK_NOPE = 64
QK_ROPE = 32
QK_HEAD = QK_NOPE + QK_ROPE
V_HEAD = 64
ATTN_WIDTH = N_HEADS * V_HEAD
ROPE_THETA = 10000.0
Q_BLOCK = 128

SSM_WIDTH = 512
SSM_GROUP = 16
SSM_GROUPS = SSM_WIDTH // SSM_GROUP
SSM_STATE = 64
SSM_CHUNK = 128
DT_MIN = 1e-3
DT_MAX = 1e-1

CONV_WIDTH = 512
CONV_K = 31

D_FF = 2816
N_EXPERTS = 8
TOP_K = 2
D_FF_EXPERT = 3584
MOE_BLOCK = 128
N_DENSE = (DEPTH + 1) // 2
N_MOE = DEPTH // 2

RMS_EPS = 1e-6
LN_EPS = 1e-5

IN_SPLITS = (Q_LORA, KV_LORA, QK_ROPE, SSM_WIDTH, CONV_WIDTH, CONV_WIDTH, 3 * D_MODEL)
D_IN = sum(IN_SPLITS)
IN_OFFSETS = tuple(int(v) for v in np.cumsum(IN_SPLITS)[:-1])

kernel_name = 'hybrid_gated_s5_mla_conformer_moe'


def rmsnorm(x, g):
    xf = x.astype(jnp.float32)
    y = xf * lax.rsqrt(jnp.mean(xf * xf, axis=-1, keepdims=True) + RMS_EPS)
    return (y * g.astype(jnp.float32)).astype(x.dtype)


def layernorm(x, g, b):
    xf = x.astype(jnp.float32)
    mu = jnp.mean(xf, axis=-1, keepdims=True)
    var = jnp.mean(jnp.square(xf - mu), axis=-1, keepdims=True)
    y = (xf - mu) * lax.rsqrt(var + LN_EPS)
    return (y * g.astype(jnp.float32) + b.astype(jnp.float32)).astype(x.dtype)


def rope_tables(positions):
    inv_freq = ROPE_THETA ** (-jnp.arange(0, QK_ROPE, 2, dtype=jnp.float32) / QK_ROPE)
    ang = positions.astype(jnp.float32)[..., None] * inv_freq
    return jnp.cos(ang)[:, :, None, :], jnp.sin(ang)[:, :, None, :]


def apply_rope(t, cos, sin):
    tf = t.astype(jnp.float32)
    half = QK_ROPE // 2
    t1, t2 = tf[..., :half], tf[..., half:]
    return jnp.concatenate([t1 * cos - t2 * sin, t1 * sin + t2 * cos], axis=-1).astype(t.dtype)


def causal_attention(q, k, v):
    B, L, H, _ = q.shape
    nb = L // Q_BLOCK
    scale = QK_HEAD ** -0.5
    qb = q.reshape(B, nb, Q_BLOCK, H, QK_HEAD).transpose(1, 0, 2, 3, 4)
    kpos = jnp.arange(L)
    neg = jnp.finfo(jnp.float32).min

    def one_block(args):
        qi, i = args
        s = jnp.einsum('bqhd,bkhd->bhqk', qi, k).astype(jnp.float32) * scale
        qpos = i * Q_BLOCK + jnp.arange(Q_BLOCK)
        mask = kpos[None, :] <= qpos[:, None]
        p = jax.nn.softmax(jnp.where(mask[None, None], s, neg), axis=-1)
        return jnp.einsum('bhqk,bkhd->bqhd', p.astype(v.dtype), v)

    out = lax.map(one_block, (qb, jnp.arange(nb)))
    return out.transpose(1, 0, 2, 3, 4).reshape(B, L, H * V_HEAD)


def mla_branch(cq, ckv, kpe, cos, sin, q_norm, w_uq, kv_norm, w_ukv):
    B, L, _ = cq.shape
    q = (rmsnorm(cq, q_norm) @ w_uq).reshape(B, L, N_HEADS, QK_HEAD)
    q = jnp.concatenate([q[..., :QK_NOPE], apply_rope(q[..., QK_NOPE:], cos, sin)], axis=-1)
    kv = (rmsnorm(ckv, kv_norm) @ w_ukv).reshape(B, L, N_HEADS, QK_NOPE + V_HEAD)
    k_nope, v = kv[..., :QK_NOPE], kv[..., QK_NOPE:]
    k_pe = apply_rope(kpe[:, :, None, :], cos, sin)
    k = jnp.concatenate([k_nope, jnp.broadcast_to(k_pe, (B, L, N_HEADS, QK_ROPE))], axis=-1)
    return causal_attention(q, k, v)


def _cmul_combine(e1, e2):
    a1r, a1i, b1r, b1i = e1
    a2r, a2i, b2r, b2i = e2
    return (a2r * a1r - a2i * a1i,
            a2r * a1i + a2i * a1r,
            a2r * b1r - a2i * b1i + b2r,
            a2r * b1i + a2i * b1r + b2i)


def s5_branch(u, lam_re, lam_im, log_dt, b_re, b_im, c_re, c_im, d_skip):
    f32 = jnp.float32
    B, L, W = u.shape
    uf = u.astype(f32)
    lr = jnp.minimum(lam_re.astype(f32), -1e-4)
    li = lam_im.astype(f32)
    dt = jnp.exp(log_dt.astype(f32))[:, None]
    mag = jnp.exp(lr * dt)
    a_re, a_im = mag * jnp.cos(li * dt), mag * jnp.sin(li * dt)
    den = lr * lr + li * li
    nr, ni = a_re - 1.0, a_im
    coef_re = (nr * lr + ni * li) / den
    coef_im = (ni * lr - nr * li) / den
    br, bi = b_re.astype(f32), b_im.astype(f32)
    bb_re = coef_re[..., None] * br - coef_im[..., None] * bi
    bb_im = coef_re[..., None] * bi + coef_im[..., None] * br
    cr, ci = c_re.astype(f32), c_im.astype(f32)
    steps = jnp.arange(1, SSM_CHUNK + 1, dtype=f32)[:, None, None]
    pmag = jnp.exp(lr[None] * dt[None] * steps)
    pw_re = pmag * jnp.cos(li[None] * dt[None] * steps)
    pw_im = pmag * jnp.sin(li[None] * dt[None] * steps)
    nc = L // SSM_CHUNK
    uc = uf.reshape(B, nc, SSM_CHUNK, SSM_GROUPS, SSM_GROUP).transpose(1, 0, 2, 3, 4)

    def chunk_step(h, u_c):
        h_re, h_im = h
        bu_re = jnp.einsum('bcgi,gpi->bcgp', u_c, bb_re)
        bu_im = jnp.einsum('bcgi,gpi->bcgp', u_c, bb_im)
        ar = jnp.broadcast_to(a_re, bu_re.shape)
        ai = jnp.broadcast_to(a_im, bu_re.shape)
        _, _, s_re, s_im = lax.associative_scan(_cmul_combine, (ar, ai, bu_re, bu_im), axis=1)
        hr, hi = h_re[:, None], h_im[:, None]
        s_re = s_re + pw_re * hr - pw_im * hi
        s_im = s_im + pw_re * hi + pw_im * hr
        y = jnp.einsum('bcgp,gop->bcgo', s_re, cr) - jnp.einsum('bcgp,gop->bcgo', s_im, ci)
        return (s_re[:, -1], s_im[:, -1]), y

    h0 = jnp.zeros((B, SSM_GROUPS, SSM_STATE), f32)
    _, y = lax.scan(chunk_step, (h0, h0), uc)
    y = y.transpose(1, 0, 2, 3, 4).reshape(B, L, W) + uf * d_skip.astype(f32)
    return y.astype(u.dtype)


def conformer_conv_branch(a, gate, w_dw, b_dw, ln_g, ln_b):
    z = a * jax.nn.sigmoid(gate)
    y = lax.conv_general_dilated(z, w_dw[:, None, :].astype(z.dtype), window_strides=(1,),
                                 padding=((CONV_K - 1, 0),),
                                 dimension_numbers=('NWC', 'WIO', 'NWC'),
                                 feature_group_count=CONV_WIDTH)
    y = layernorm(y + b_dw, ln_g, ln_b)
    return jax.nn.silu(y)


def swiglu(h, w1, w3, w2):
    return (jax.nn.silu(h @ w1) * (h @ w3)) @ w2


def moe_swiglu(h, w_router, w1, w3, w2):
    B, L, D = h.shape
    T = B * L
    xt = h.reshape(T, D)
    logits = (xt @ w_router).astype(jnp.float32)
    top_val, top_idx = lax.top_k(logits, TOP_K)
    gate = jax.nn.softmax(top_val, axis=-1)
    M = T * TOP_K
    e_flat = top_idx.reshape(M)
    g_flat = gate.reshape(M)
    tok_flat = jnp.repeat(jnp.arange(T, dtype=jnp.int32), TOP_K, total_repeat_length=M)
    order = jnp.argsort(e_flat)
    e_sorted = e_flat[order]
    counts = jax.ops.segment_sum(jnp.ones((M,), jnp.int32), e_flat, num_segments=N_EXPERTS)
    starts = jnp.cumsum(counts) - counts
    padded = ((counts + MOE_BLOCK - 1) // MOE_BLOCK) * MOE_BLOCK
    pad_ends = jnp.cumsum(padded)
    pad_starts = pad_ends - padded
    dest = pad_starts[e_sorted] + (jnp.arange(M) - starts[e_sorted])
    n_blocks = -(-M // MOE_BLOCK) + N_EXPERTS
    P = n_blocks * MOE_BLOCK
    tok_buf = jnp.zeros((P,), jnp.int32).at[dest].set(tok_flat[order])
    gate_buf = jnp.zeros((P,), jnp.float32).at[dest].set(g_flat[order])
    block_start = jnp.arange(n_blocks) * MOE_BLOCK
    block_e = jnp.minimum(jnp.searchsorted(pad_ends, block_start, side='right'), N_EXPERTS - 1)
    x_buf = xt[tok_buf].reshape(n_blocks, MOE_BLOCK, D)

    def expert_block(args):
        xb, e = args
        return (jax.nn.silu(xb @ w1[e]) * (xb @ w3[e])) @ w2[e]

    y_buf = lax.map(expert_block, (x_buf, block_e)).reshape(P, D)
    y = jnp.zeros((T, D), h.dtype).at[tok_buf].add(y_buf * gate_buf[:, None].astype(h.dtype))
    return y.reshape(B, L, D)


def setup_inputs(seed: int = 0) -> dict:
    key = jax.random.key(seed)
    ks = iter(jax.random.split(key, 48))
    f32 = jnp.float32

    def nrm(shape, scale):
        return jax.random.normal(next(ks), shape, f32) * scale

    def gain(shape):
        return 1.0 + 0.02 * jax.random.normal(next(ks), shape, f32)

    x = jax.random.normal(next(ks), (BATCH, SEQ, D_MODEL), f32)
    offs = jax.random.randint(next(ks), (BATCH, 1), 0, 1024, dtype=jnp.int32)
    positions = offs + jnp.arange(SEQ, dtype=jnp.int32)[None, :]
    G, P, I = SSM_GROUPS, SSM_STATE, SSM_GROUP
    n_idx = jnp.arange(P, dtype=f32)
    return {
        'x': x,
        'positions': positions,
        'norm_mix': gain((DEPTH, D_MODEL)),
        'w_in': nrm((DEPTH, D_MODEL, D_IN), D_MODEL ** -0.5),
        'q_norm': gain((DEPTH, Q_LORA)),
        'w_uq': nrm((DEPTH, Q_LORA, N_HEADS * QK_HEAD), Q_LORA ** -0.5),
        'kv_norm': gain((DEPTH, KV_LORA)),
        'w_ukv': nrm((DEPTH, KV_LORA, N_HEADS * (QK_NOPE + V_HEAD)), KV_LORA ** -0.5),
        'w_o_attn': nrm((DEPTH, ATTN_WIDTH, D_MODEL), ATTN_WIDTH ** -0.5),
        'ssm_lam_re': -0.5 + nrm((DEPTH, G, P), 0.01),
        'ssm_lam_im': jnp.pi * n_idx + nrm((DEPTH, G, P), 0.01),
        'ssm_log_dt': jax.random.uniform(next(ks), (DEPTH, G), f32, np.log(DT_MIN), np.log(DT_MAX)),
        'ssm_b_re': nrm((DEPTH, G, P, I), (2.0 * I) ** -0.5),
        'ssm_b_im': nrm((DEPTH, G, P, I), (2.0 * I) ** -0.5),
        'ssm_c_re': nrm((DEPTH, G, I, P), (2.0 * P) ** -0.5),
        'ssm_c_im': nrm((DEPTH, G, I, P), (2.0 * P) ** -0.5),
        'ssm_d': nrm((DEPTH, SSM_WIDTH), 1.0),
        'ssm_w_glu': nrm((DEPTH, SSM_WIDTH, SSM_WIDTH), SSM_WIDTH ** -0.5),
        'ssm_b_glu': nrm((DEPTH, SSM_WIDTH), 0.01),
        'w_o_ssm': nrm((DEPTH, SSM_WIDTH, D_MODEL), SSM_WIDTH ** -0.5),
        'conv_w': nrm((DEPTH, CONV_K, CONV_WIDTH), CONV_K ** -0.5),
        'conv_b': nrm((DEPTH, CONV_WIDTH), 0.01),
        'conv_ln_g': gain((DEPTH, CONV_WIDTH)),
        'conv_ln_b': nrm((DEPTH, CONV_WIDTH), 0.01),
        'w_o_conv': nrm((DEPTH, CONV_WIDTH, D_MODEL), CONV_WIDTH ** -0.5),
        'w_out': nrm((DEPTH, D_MODEL, D_MODEL), D_MODEL ** -0.5),
        'norm_ffn': gain((DEPTH, D_MODEL)),
        'ffn_w1': nrm((N_DENSE, D_MODEL, D_FF), D_MODEL ** -0.5),
        'ffn_w3': nrm((N_DENSE, D_MODEL, D_FF), D_MODEL ** -0.5),
        'ffn_w2': nrm((N_DENSE, D_FF, D_MODEL), D_FF ** -0.5),
        'moe_router': nrm((N_MOE, D_MODEL, N_EXPERTS), D_MODEL ** -0.5),
        'moe_w1': nrm((N_MOE, N_EXPERTS, D_MODEL, D_FF_EXPERT), D_MODEL ** -0.5),
        'moe_w3': nrm((N_MOE, N_EXPERTS, D_MODEL, D_FF_EXPERT), D_MODEL ** -0.5),
        'moe_w2': nrm((N_MOE, N_EXPERTS, D_FF_EXPERT, D_MODEL), D_FF_EXPERT ** -0.5),
        'norm_final': gain((D_MODEL,)),
    }


def reference(x, positions, norm_mix, w_in, q_norm, w_uq, kv_norm, w_ukv, w_o_attn,
              ssm_lam_re, ssm_lam_im, ssm_log_dt, ssm_b_re, ssm_b_im, ssm_c_re, ssm_c_im,
              ssm_d, ssm_w_glu, ssm_b_glu, w_o_ssm,
              conv_w, conv_b, conv_ln_g, conv_ln_b, w_o_conv,
              w_out, norm_ffn, ffn_w1, ffn_w3, ffn_w2,
              moe_router, moe_w1, moe_w3, moe_w2, norm_final):
    B, L, D = x.shape
    cos, sin = rope_tables(positions)
    for layer in range(DEPTH):
        h = rmsnorm(x, norm_mix[layer])
        z = h @ w_in[layer]
        cq, ckv, kpe, u_ssm, conv_a, conv_g, gates = jnp.split(z, IN_OFFSETS, axis=-1)
        y_attn = mla_branch(cq, ckv, kpe, cos, sin, q_norm[layer], w_uq[layer],
                            kv_norm[layer], w_ukv[layer]) @ w_o_attn[layer]
        s = s5_branch(u_ssm, ssm_lam_re[layer], ssm_lam_im[layer], ssm_log_dt[layer],
                      ssm_b_re[layer], ssm_b_im[layer], ssm_c_re[layer], ssm_c_im[layer],
                      ssm_d[layer])
        s = jax.nn.gelu(s)
        s = s * jax.nn.sigmoid(s @ ssm_w_glu[layer] + ssm_b_glu[layer])
        y_ssm = s @ w_o_ssm[layer]
        y_conv = conformer_conv_branch(conv_a, conv_g, conv_w[layer], conv_b[layer],
                                       conv_ln_g[layer], conv_ln_b[layer]) @ w_o_conv[layer]
        g = jax.nn.sigmoid(gates).reshape(B, L, 3, D)
        merged = g[:, :, 0] * y_attn + g[:, :, 1] * y_ssm + g[:, :, 2] * y_conv
        x = x + merged @ w_out[layer]
        h = rmsnorm(x, norm_ffn[layer])
        if layer % 2 == 0:
            i = layer // 2
            x = x + swiglu(h, ffn_w1[i], ffn_w3[i], ffn_w2[i])
        else:
            i = layer // 2
            x = x + moe_swiglu(h, moe_router[i], moe_w1[i], moe_w3[i], moe_w2[i])
    return rmsnorm(x, norm_final)
```

```python
import numpy as np
from contextlib import ExitStack
import concourse.bass as bass
import concourse.mybir as mybir
from concourse.bass_utils import run_bass_kernel_spmd

F32 = mybir.dt.float32
BF16 = mybir.dt.bfloat16
I32 = mybir.dt.int32
AF = mybir.ActivationFunctionType
ALU = mybir.AluOpType


class Buf:
    def __init__(self, kb, name, t, space):
        self.kb = kb
        self.name = name
        self.t = t
        self.space = space
        self.w = {}
        self.reads = {}
        self.ld_sem = None
        self.st_sem = None

    def __getitem__(self, idx):
        return self.t[idx]

    def ap(self):
        return self.t.ap()

    def note_write(self, key, val):
        if self.space == "dram":
            self.w[key] = max(self.w.get(key, -1), val)
        else:
            self.w = {key: val}
            self.reads = {}

    def note_read(self, key, val):
        self.reads[key] = max(self.reads.get(key, -1), val)


class KB:
    ENGS = ("tensor", "vector", "scalar", "gpsimd", "sync")
    EPOCH = 8192

    def __init__(self, nc, same_engine_sync=True):
        self.nc = nc
        self.es = ExitStack()
        self.pes = None
        self.pfx = ""
        self.streams = {e: [] for e in self.ENGS}
        self.count = {e: 0 for e in self.ENGS}
        self.sems = {}
        self.semval = {}
        self.waited = {e: {} for e in self.ENGS}
        self.same_engine_sync = same_engine_sync
        self.ncoll = 0

    def _sem(self, key):
        if key not in self.sems:
            nm = "s_" + "_".join(str(k) for k in key)
            self.sems[key] = self.es.enter_context(self.nc.semaphore(nm))
            self.semval[key] = 0
        return self.sems[key]

    def _stack(self):
        return self.pes if self.pes is not None else self.es

    def sb(self, name, shape, dtype=F32):
        name = self.pfx + name
        t = self._stack().enter_context(self.nc.sbuf_tensor(name, list(shape), dtype))
        return Buf(self, name, t, "sb")

    def ps(self, name, shape=(128, 512), dtype=F32):
        name = self.pfx + name
        t = self._stack().enter_context(self.nc.psum_tensor(name, list(shape), dtype))
        return Buf(self, name, t, "ps")

    def dram(self, name, shape, dtype, kind):
        return self.nc.dram_tensor(name, list(shape), dtype, kind=kind).ap()

    def dbuf(self, name, shape, dtype, kind="Internal"):
        t = self.nc.dram_tensor(name, list(shape), dtype, kind=kind)
        return Buf(self, name, t, "dram")

    def _need(self, eng, key, val, waits):
        if key[0] == "eng":
            e2, idx = key[1], val
            if e2 == eng and (eng == "tensor" or not self.same_engine_sync):
                return
            key, val = ("eng", e2, idx // self.EPOCH), idx % self.EPOCH + 1
        if self.waited[eng].get(key, 0) >= val:
            return
        waits[key] = max(waits.get(key, 0), val)

    def _deps(self, eng, reads, writes):
        waits = {}
        for b in reads:
            for k, v in b.w.items():
                self._need(eng, k, v, waits)
        for b in writes:
            for k, v in b.w.items():
                self._need(eng, k, v, waits)
            for k, v in b.reads.items():
                self._need(eng, k, v, waits)
        for key, val in waits.items():
            self.waited[eng][key] = val
        return [(self.sems[k], v) for k, v in waits.items()]

    def op(self, eng, fn, reads=(), writes=()):
        waits = self._deps(eng, reads, writes)
        idx = self.count[eng]
        self.count[eng] += 1
        sem = self._sem(("eng", eng, idx // self.EPOCH))

        def emit(e, fn=fn, waits=waits, sem=sem):
            for s, v in waits:
                e.wait_ge(s, v)
            fn(e).then_inc(sem, 1)

        self.streams[eng].append(emit)
        for b in reads:
            b.note_read(("eng", eng), idx)
        for b in writes:
            b.note_write(("eng", eng), idx)
        return idx

    def dma(self, queue, out_ap, in_ap, reads=(), writes=(), **kw):
        waits = self._deps(queue, reads, writes)
        sbw = [b for b in writes if b.space != "dram"]
        sbr = [b for b in reads if b.space != "dram"]
        assert len(sbw) + len(sbr) <= 1
        if sbw:
            b = sbw[0]
            if b.ld_sem is None:
                b.ld_sem = ("ld", b.name)
            key = b.ld_sem
        elif sbr:
            b = sbr[0]
            if b.st_sem is None:
                b.st_sem = ("st", b.name)
            key = b.st_sem
        else:
            key = ("dd", writes[0].name)
        sem = self._sem(key)
        self.semval[key] += 16
        val = self.semval[key]

        def emit(e, waits=waits, sem=sem, out_ap=out_ap, in_ap=in_ap, kw=kw):
            for s, v in waits:
                e.wait_ge(s, v)
            e.dma_start(out=out_ap, in_=in_ap, **kw).then_inc(sem, 16)

        self.streams[queue].append(emit)
        for b in writes:
            b.note_write(key, val)
        for b in reads:
            b.note_read(key, val)
        return key, val

    def coll(self, kind, ins, outs, groups):
        waits = self._deps("gpsimd", ins, outs)
        key = ("cc", self.ncoll)
        self.ncoll += 1
        sem = self._sem(key)
        self.semval[key] = 16
        in_aps = [b.ap() for b in ins]
        out_aps = [b.ap() for b in outs]

        def emit(e, waits=waits, sem=sem):
            for s, v in waits:
                e.wait_ge(s, v)
            e.collective_compute(kind, ALU.bypass, replica_groups=groups, ins=in_aps, outs=out_aps).then_inc(sem, 16)

        self.streams["gpsimd"].append(emit)
        for b in outs:
            b.note_write(key, 16)
        for b in ins:
            b.note_read(key, 16)

    def final_wait(self, queue, bufs):
        waits = []
        for b in bufs:
            if b.st_sem is not None:
                waits.append((self.sems[b.st_sem], self.semval[b.st_sem]))

        def emit(e, waits=waits):
            for s, v in waits:
                e.wait_ge(s, v)

        self.streams[queue].append(emit)

    def phase_begin(self, pfx):
        self.pes = ExitStack()
        self.pfx = pfx

    def phase_end(self):
        barrier(self)
        self.flush()
        self.pes.close()
        self.pes = None
        self.pfx = ""

    def flush(self):
        nc = self.nc
        with nc.Block() as block:
            for ename in self.ENGS:
                stream = self.streams[ename]
                if not stream:
                    continue

                def body(e, stream=stream):
                    for emit in stream:
                        emit(e)

                getattr(block, ename)(body)
        self.streams = {e: [] for e in self.ENGS}

    def build(self):
        self.flush()
        if self.pes is not None:
            self.pes.close()
        self.es.close()
        return self.nc


def _add_helpers():
    def mm(self, ps, out, lhsT, rhs, start, stop, reads):
        return self.op("tensor", lambda e: e.matmul(out, lhsT, rhs, start=start, stop=stop), reads=reads, writes=[ps])

    def act(self, out, in_, func, reads, writes, bias=None, scale=None):
        kw = {}
        if bias is not None:
            kw["bias"] = bias
        if scale is not None:
            kw["scale"] = scale
        return self.op("scalar", lambda e: e.activation(out, in_, func, **kw), reads=reads, writes=writes)

    def tt(self, eng, out, in0, in1, op, reads, writes):
        return self.op(eng, lambda e: e.tensor_tensor(out, in0, in1, op=op), reads=reads, writes=writes)

    def ts(self, eng, out, in0, s1, s2, op0, op1, reads, writes):
        if op1 is None:
            return self.op(eng, lambda e: e.tensor_scalar(out, in0, s1, None, op0=op0), reads=reads, writes=writes)
        return self.op(eng, lambda e: e.tensor_scalar(out, in0, s1, s2, op0=op0, op1=op1), reads=reads, writes=writes)

    def stt(self, out, in0, scalar, in1, op0, op1, reads, writes):
        return self.op("vector", lambda e: e.scalar_tensor_tensor(out, in0, scalar, in1, op0=op0, op1=op1), reads=reads, writes=writes)

    def copy(self, eng, out, in_, reads, writes):
        if eng == "scalar":
            return self.op("scalar", lambda e: e.copy(out, in_), reads=reads, writes=writes)
        return self.op(eng, lambda e: e.tensor_copy(out, in_), reads=reads, writes=writes)

    def recip(self, out, in_, reads, writes):
        return self.op("vector", lambda e: e.reciprocal(out, in_), reads=reads, writes=writes)

    def memset(self, eng, out, val, writes):
        return self.op(eng, lambda e: e.memset(out, val), writes=writes)

    for f in (mm, act, tt, ts, stt, copy, recip, memset):
        setattr(KB, f.__name__, f)


_add_helpers()


D = 1024
SEQ = 16384
NTOK = 4096
TT = 512
EPS = 1e-6
WA_COLS = 1984
TWO_PI = 6.283185307179586
MUL, ADD = ALU.mult, ALU.add


class Pool:
    def __init__(self, bufs):
        self.bufs = bufs
        self.i = 0

    def next(self):
        b = self.bufs[self.i % len(self.bufs)]
        self.i += 1
        return b


def load_cast(kb, dst, dst_aps, src_aps, stage_pool, engs=("scalar", "gpsimd", "vector")):
    for k, (d, src) in enumerate(zip(dst_aps, src_aps)):
        st = stage_pool.next()
        shp = src.shape
        sview = st[:shp[0], :shp[1]]
        kb.dma("sync", sview, src, writes=[st])
        kb.copy(engs[k % len(engs)], d, sview, reads=[st], writes=[dst])


def make_consts(kb, eps=EPS):
    kb.ones_bf = kb.sb("ones_bf", [128, 128], BF16)
    kb.memset("vector", kb.ones_bf[:], 1.0, writes=[kb.ones_bf])
    kb.eps_t = kb.sb("eps_t", [128, 1], F32)
    kb.memset("vector", kb.eps_t[:], eps, writes=[kb.eps_t])


def rms_stats(kb, pp, src, nk, width, sq, rstd, tmp, inv_n):
    kb.act(sq[:, :nk, :width], src[:, :nk, :width], AF.Square, reads=[src], writes=[sq])
    ps = pp.next()
    for k in range(nk):
        kb.mm(ps, ps[:, :width], kb.ones_bf[:], sq[:, k, :width], k == 0, k == nk - 1, reads=[kb.ones_bf, sq])
    kb.act(tmp[:, :width], ps[:, :width], AF.Sqrt, reads=[ps, kb.eps_t], writes=[tmp], bias=kb.eps_t[:, 0:1], scale=inv_n)
    kb.recip(rstd[:, :width], tmp[:, :width], reads=[tmp], writes=[rstd])


def rope_scratch(kb, n, pfx=""):
    return dict(posi=kb.sb(pfx + "posi", [128, n], I32), ang=kb.sb(pfx + "ang", [128, n], F32), kk=kb.sb(pfx + "kk", [128, n], F32),
                ki=kb.sb(pfx + "ki", [128, n], I32), msk=kb.sb(pfx + "msk", [128, n], F32))


def sincos(kb, ang, SIN, COS, scr):
    kk, ki, msk = scr["kk"], scr["ki"], scr["msk"]
    V = "vector"
    kb.ts(V, kk[:], ang[:], 1.0 / TWO_PI, None, MUL, None, reads=[ang], writes=[kk])
    kb.copy(V, ki[:], kk[:], reads=[kk], writes=[ki])
    kb.copy(V, kk[:], ki[:], reads=[ki], writes=[kk])
    C1 = 6.28125
    C2 = TWO_PI - C1
    kb.stt(ang[:], kk[:], -C1, ang[:], MUL, ADD, reads=[kk, ang], writes=[ang])
    kb.stt(ang[:], kk[:], -C2, ang[:], MUL, ADD, reads=[kk, ang], writes=[ang])

    def wrap(buf):
        kb.ts(V, msk[:], buf[:], float(np.pi), -TWO_PI, ALU.is_gt, MUL, reads=[buf], writes=[msk])
        kb.tt(V, buf[:], buf[:], msk[:], ADD, reads=[buf, msk], writes=[buf])
        kb.ts(V, msk[:], buf[:], float(-np.pi), TWO_PI, ALU.is_lt, MUL, reads=[buf], writes=[msk])
        kb.tt(V, buf[:], buf[:], msk[:], ADD, reads=[buf, msk], writes=[buf])

    wrap(ang)
    kb.act(SIN[:], ang[:], AF.Sin, reads=[ang], writes=[SIN])
    kb.ts(V, ang[:], ang[:], float(np.pi / 2), None, ADD, None, reads=[ang], writes=[ang])
    wrap(ang)
    kb.act(COS[:], ang[:], AF.Sin, reads=[ang], writes=[COS])


def rope_tables(kb, pos_ap, rc, COS, SINS, scr):
    posi, ang = scr["posi"], scr["ang"]
    V = "vector"
    kb.dma("sync", posi[:], pos_ap.partition_broadcast(128), writes=[posi])
    kb.copy(V, ang[:], posi[:], reads=[posi], writes=[ang])
    kb.ts(V, ang[:], ang[:], rc[:, 0:1], None, MUL, None, reads=[ang, rc], writes=[ang])
    sincos(kb, ang, SINS, COS, scr)
    kb.ts(V, SINS[:], SINS[:], rc[:, 1:2], None, MUL, None, reads=[SINS, rc], writes=[SINS])


def build_A():
    nc = bass.Bass("TRN2", target_bir_lowering=False)
    kb = KB(nc)
    xT = kb.dram("xT", [D, NTOK], F32, "ExternalInput")
    pos = kb.dram("pos", [1, NTOK], I32, "ExternalInput")
    gmix = kb.dram("gmix", [128, 8], F32, "ExternalInput")
    WA = kb.dram("WA", [D, WA_COLS], F32, "ExternalInput")
    qn = kb.dram("qn", [128, 2], F32, "ExternalInput")
    WQ = kb.dram("WQ", [256, 8 * 96], F32, "ExternalInput")
    WQS = kb.dram("WQS", [256, 8 * 32], F32, "ExternalInput")
    kvn = kb.dram("kvn", [128, 1], F32, "ExternalInput")
    WK = kb.dram("WK", [128, 512], F32, "ExternalInput")
    WV = kb.dram("WV", [128, 512], F32, "ExternalInput")
    ropec = kb.dram("ropec", [128, 2], F32, "ExternalInput")
    qT = kb.dram("qT", [8 * 96, NTOK], BF16, "ExternalOutput")
    kT = kb.dram("kT", [8 * 96, NTOK], BF16, "ExternalOutput")
    Vo = kb.dram("V", [NTOK, 512], BF16, "ExternalOutput")
    uT = kb.dram("uT", [512, NTOK], F32, "ExternalOutput")
    zcT = kb.dram("zcT", [512, NTOK], F32, "ExternalOutput")

    make_consts(kb)
    pp = Pool([kb.ps(f"ps{i}") for i in range(8)])
    stage = Pool([kb.sb(f"stg{i}", [128, WA_COLS], F32) for i in range(2)])

    wa = kb.sb("wa", [128, 8, WA_COLS], BF16)
    load_cast(kb, wa, [wa[:, k, :] for k in range(8)], [WA[k * 128:(k + 1) * 128, :] for k in range(8)], stage)
    wq = kb.sb("wq", [128, 2, 768], BF16)
    load_cast(kb, wq, [wq[:, k, :] for k in range(2)], [WQ[k * 128:(k + 1) * 128, :] for k in range(2)], stage)
    wqs = kb.sb("wqs", [128, 2, 256], BF16)
    load_cast(kb, wqs, [wqs[:, k, :] for k in range(2)], [WQS[k * 128:(k + 1) * 128, :] for k in range(2)], stage)
    wk = kb.sb("wk", [128, 512], BF16)
    load_cast(kb, wk, [wk[:, :]], [WK[:, :]], stage)
    wv = kb.sb("wv", [128, 512], BF16)
    load_cast(kb, wv, [wv[:, :]], [WV[:, :]], stage)
    gm = kb.sb("gm", [128, 8], F32)
    kb.dma("sync", gm[:], gmix[:, :], writes=[gm])
    qn_s = kb.sb("qn_s", [128, 2], F32)
    kb.dma("sync", qn_s[:], qn[:, :], writes=[qn_s])
    kvn_s = kb.sb("kvn_s", [128, 1], F32)
    kb.dma("sync", kvn_s[:], kvn[:, :], writes=[kvn_s])
    rc = kb.sb("rc", [128, 2], F32)
    kb.dma("sync", rc[:], ropec[:, :], writes=[rc])

    COS = kb.sb("COS", [128, TT], F32)
    SINS = kb.sb("SINS", [128, TT], F32)
    scr = rope_scratch(kb, TT)

    xs_pool = Pool([kb.sb(f"xs{i}", [128, 8, TT], F32) for i in range(2)])
    sq = kb.sb("sq", [128, 8, TT], BF16)
    h = kb.sb("h", [128, 8, TT], BF16)
    rstd = kb.sb("rstd", [128, TT], F32)
    tmp = kb.sb("tmp", [128, TT], F32)
    cq = kb.sb("cq", [128, 2, TT], F32)
    cqn = kb.sb("cqn", [128, 2, TT], BF16)
    ckv = kb.sb("ckv", [128, 1, TT], F32)
    ckvn = kb.sb("ckvn", [128, TT], BF16)
    kpe = kb.sb("kpe", [128, TT], BF16)
    t1 = kb.sb("t1", [128, TT], F32)
    t2 = kb.sb("t2", [128, TT], F32)
    qo_pool = Pool([kb.sb(f"qo{i}", [128, TT], BF16) for i in range(3)])
    ko_pool = Pool([kb.sb(f"ko{i}", [128, TT], BF16) for i in range(3)])
    vo_pool = Pool([kb.sb(f"vo{i}", [128, 512], BF16) for i in range(2)])
    fo_pool = Pool([kb.sb(f"fo{i}", [128, TT], F32) for i in range(3)])
    sg = kb.sb("sg", [128, TT], F32)
    V, G, S = "vector", "gpsimd", "scalar"

    xv = xT.rearrange("(k p) t -> p k t", p=128)
    for it in range(NTOK // TT):
        c0 = it * TT
        cs = slice(c0, c0 + TT)
        xs = xs_pool.next()
        kb.dma("sync", xs[:], xv[:, :, cs], writes=[xs])
        rope_tables(kb, pos[0:1, cs], rc, COS, SINS, scr)
        rms_stats(kb, pp, xs, 8, TT, sq, rstd, tmp, 1.0 / D)
        for k in range(8):
            kb.stt(h[:, k, :], xs[:, k, :], gm[:, k:k + 1], rstd[:], MUL, MUL, reads=[xs, gm, rstd], writes=[h])

        def proj(ps, out_ap, col0, ncol):
            for k in range(8):
                kb.mm(ps, out_ap, wa[:, k, col0:col0 + ncol], h[:, k, :], k == 0, k == 7, reads=[wa, h])

        for m in range(2):
            ps = pp.next()
            proj(ps, ps[:, :], m * 128, 128)
            kb.copy(S, cq[:, m, :], ps[:, :], reads=[ps], writes=[cq])
        rms_stats(kb, pp, cq, 2, TT, sq, rstd, tmp, 1.0 / 256)
        for m in range(2):
            kb.stt(cqn[:, m, :], cq[:, m, :], qn_s[:, m:m + 1], rstd[:], MUL, MUL, reads=[cq, qn_s, rstd], writes=[cqn])
        for hh in range(8):
            psq = pp.next()
            pss = pp.next()
            for m in range(2):
                kb.mm(psq, psq[0:96, :], wq[:, m, hh * 96:(hh + 1) * 96], cqn[:, m, :], m == 0, m == 1, reads=[wq, cqn])
            for m in range(2):
                kb.mm(pss, pss[64:96, :], wqs[:, m, hh * 32:(hh + 1) * 32], cqn[:, m, :], m == 0, m == 1, reads=[wqs, cqn])
            qo = qo_pool.next()
            kb.copy(S, qo[0:64, :], psq[0:64, :], reads=[psq], writes=[qo])
            kb.tt(V, t1[64:96, :], psq[64:96, :], COS[64:96, :], MUL, reads=[psq, COS], writes=[t1])
            kb.tt(V, t2[64:96, :], pss[64:96, :], SINS[64:96, :], MUL, reads=[pss, SINS], writes=[t2])
            kb.tt(G, qo[64:96, :], t1[64:96, :], t2[64:96, :], ADD, reads=[t1, t2], writes=[qo])
            kb.dma("gpsimd", qT[hh * 96:(hh + 1) * 96, cs], qo[0:96, :], reads=[qo])

        ps = pp.next()
        proj(ps, ps[:, :], 256, 128)
        kb.copy(S, ckv[:, 0, :], ps[:, :], reads=[ps], writes=[ckv])
        rms_stats(kb, pp, ckv, 1, TT, sq, rstd, tmp, 1.0 / 128)
        kb.stt(ckvn[:, :], ckv[:, 0, :], kvn_s[:, 0:1], rstd[:], MUL, MUL, reads=[ckv, kvn_s, rstd], writes=[ckvn])
        psa = pp.next()
        proj(psa, psa[64:96, :], 384, 32)
        psb = pp.next()
        proj(psb, psb[64:96, :], 416, 32)
        kb.tt(V, t1[64:96, :], psa[64:96, :], COS[64:96, :], MUL, reads=[psa, COS], writes=[t1])
        kb.tt(V, t2[64:96, :], psb[64:96, :], SINS[64:96, :], MUL, reads=[psb, SINS], writes=[t2])
        kb.tt(G, kpe[64:96, :], t1[64:96, :], t2[64:96, :], ADD, reads=[t1, t2], writes=[kpe])
        for hh in range(8):
            psk = pp.next()
            kb.mm(psk, psk[0:64, :], wk[:, hh * 64:(hh + 1) * 64], ckvn[:, :], True, True, reads=[wk, ckvn])
            ko = ko_pool.next()
            kb.copy(S, ko[0:64, :], psk[0:64, :], reads=[psk], writes=[ko])
            kb.copy(G, ko[64:96, :], kpe[64:96, :], reads=[kpe], writes=[ko])
            kb.dma("gpsimd", kT[hh * 96:(hh + 1) * 96, cs], ko[0:96, :], reads=[ko])
        for j in range(TT // 128):
            psv = pp.next()
            kb.mm(psv, psv[:, :], ckvn[:, j * 128:(j + 1) * 128], wv[:, :], True, True, reads=[wv, ckvn])
            vo = vo_pool.next()
            kb.copy(V, vo[:, :], psv[:, :], reads=[psv], writes=[vo])
            kb.dma("gpsimd", Vo[c0 + j * 128:c0 + (j + 1) * 128, :], vo[:, :], reads=[vo])

        for m in range(4):
            ps = pp.next()
            proj(ps, ps[:, :], 448 + m * 128, 128)
            fo = fo_pool.next()
            kb.copy(S, fo[:, :], ps[:, :], reads=[ps], writes=[fo])
            kb.dma("gpsimd", uT[m * 128:(m + 1) * 128, cs], fo[:, :], reads=[fo])
        for m in range(4):
            psa = pp.next()
            proj(psa, psa[:, :], 960 + m * 128, 128)
            psg = pp.next()
            proj(psg, psg[:, :], 1472 + m * 128, 128)
            kb.act(sg[:, :], psg[:, :], AF.Sigmoid, reads=[psg], writes=[sg])
            fo = fo_pool.next()
            kb.tt(V, fo[:, :], psa[:, :], sg[:, :], MUL, reads=[psa, sg], writes=[fo])
            kb.dma("gpsimd", zcT[m * 128:(m + 1) * 128, cs], fo[:, :], reads=[fo])

    kb.final_wait("gpsimd", qo_pool.bufs + ko_pool.bufs + vo_pool.bufs + fo_pool.bufs)
    return kb.build()


def _col(v, n):
    return np.ascontiguousarray(np.asarray(v, np.float32).reshape(n, 128).T)


ROPE_CONST = None


def rope_const():
    inv_freq = (10000.0 ** (-np.arange(0, 32, 2, dtype=np.float32) / 32)).astype(np.float32)
    rc = np.zeros((128, 2), np.float32)
    rc[64:80, 0] = inv_freq
    rc[80:96, 0] = inv_freq
    rc[64:80, 1] = -1.0
    rc[80:96, 1] = 1.0
    return rc


def prep_A(inp, l):
    w_in = inp["w_in"][l]
    kpe_w = w_in[:, 384:416]
    kpe_sw = np.concatenate([kpe_w[:, 16:32], kpe_w[:, 0:16]], axis=1)
    WA = np.concatenate([w_in[:, 0:384], kpe_w, kpe_sw, w_in[:, 416:416 + 512], w_in[:, 928:1440], w_in[:, 1440:1952]], axis=1)
    wuq = inp["w_uq"][l].reshape(256, 8, 96)
    WQ = wuq.reshape(256, 768)
    rp = wuq[:, :, 64:96]
    WQS = np.concatenate([rp[:, :, 16:32], rp[:, :, 0:16]], axis=2).reshape(256, 256)
    wukv = inp["w_ukv"][l].reshape(128, 8, 128)
    WK = wukv[:, :, 0:64].reshape(128, 512)
    WV = wukv[:, :, 64:128].reshape(128, 512)
    common = dict(gmix=_col(inp["norm_mix"][l], 8), WA=np.ascontiguousarray(WA), qn=_col(inp["q_norm"][l], 2),
                  WQ=np.ascontiguousarray(WQ), WQS=np.ascontiguousarray(WQS), kvn=_col(inp["kv_norm"][l], 1),
                  WK=np.ascontiguousarray(WK), WV=np.ascontiguousarray(WV), ropec=rope_const())
    return common


NCH = SEQ // 16


def barrier(kb):
    targets = []
    for e in kb.ENGS:
        n = kb.count[e]
        if n:
            idx = n - 1
            targets.append((("eng", e, idx // kb.EPOCH), idx % kb.EPOCH + 1))
    for key, v in kb.semval.items():
        if key[0] in ("ld", "st") and v:
            targets.append((key, v))
    for e in kb.ENGS:
        ws = []
        for key, v in targets:
            if key[0] == "eng" and key[1] == e:
                continue
            if kb.waited[e].get(key, 0) < v:
                kb.waited[e][key] = v
                ws.append((kb.sems[key], v))

        def emit(eng, ws=ws):
            for s, v in ws:
                eng.wait_ge(s, v)

        kb.streams[e].append(emit)


def attention_head(kb, pp_s, pp_o, qTd, kTd, Vd, OTd, hl, bufs):
    kT, qT, V, pts, outs = bufs["kT"], bufs["qT"], bufs["V"], bufs["pts"], bufs["outs"]
    scale = 96 ** -0.5
    nq = SEQ // 4096
    for i in range(nq):
        sl = slice(i * 4096, (i + 1) * 4096)
        kb.dma("sync", kT[0:96, sl], kTd[hl * 96:(hl + 1) * 96, sl], writes=[kT])
    Vv = Vd.rearrange("(n p) f -> p n f", p=128)
    for i in range(4):
        kb.dma("sync", V[:, i * 32:(i + 1) * 32, 0:64], Vv[:, i * 32:(i + 1) * 32, hl * 64:(hl + 1) * 64], writes=[V])
    kb.memset("vector", V[:, :, 64:65], 1.0, writes=[V])

    blocks = []
    for qc in range(SEQ // 512):
        nkb = 4 * (qc + 1)
        for i in range(nkb):
            j = i - 4 * qc
            lo = 128 * j if j > 0 else 0
            blocks.append((qc, i, lo, j >= 0, i == 0, i == nkb - 1))
    LOOK = 2
    sps = {}
    ops_ = {}
    qts = {}

    def issue_s(n):
        qc, i, lo, diag, first, last = blocks[n]
        ps = pp_s.next()
        sps[n] = ps
        if first:
            qt = qT.next()
            qts[qc] = qt
            kb.dma("sync", qt[0:96, :], qTd[hl * 96:(hl + 1) * 96, qc * 512:(qc + 1) * 512], writes=[qt])
        qt = qts[qc]
        kb.mm(ps, ps[:, lo:512], kT[0:96, i * 128:(i + 1) * 128], qt[0:96, lo:512], True, True, reads=[kT, qt])

    for n in range(min(LOOK, len(blocks))):
        issue_s(n)
    for n, (qc, i, lo, diag, first, last) in enumerate(blocks):
        if n + LOOK < len(blocks):
            issue_s(n + LOOK)
        ps = sps.pop(n)
        pt = pts.next()
        kb.act(pt[:, lo:512], ps[:, lo:512], AF.Exp, reads=[ps], writes=[pt], scale=scale)
        if diag:
            kb.op("gpsimd", lambda e, pt=pt, lo=lo: e.affine_select(pt[:, lo:lo + 128], pt[:, lo:lo + 128], [[1, 128]], ALU.is_ge, 0.0,
                                                                    base=0, channel_multiplier=-1), reads=[pt], writes=[pt])
        if first:
            ops_[qc] = pp_o.next()
        po = ops_[qc]
        kb.mm(po, po[0:65, lo:512], V[:, i, :], pt[:, lo:512], first, last, reads=[V, pt])
        if last:
            o = outs.next()
            kb.copy("vector", o[0:65, :], po[0:65, :], reads=[po], writes=[o])
            kb.dma("gpsimd", OTd[hl * 65:(hl + 1) * 65, qc * 512:(qc + 1) * 512], o[0:65, :], reads=[o])


def conv_phase(kb, pp, zcTd, cwTd, cbd, identd, ycTd, bufs):
    stage, ident, outs = bufs["stage"], bufs["ident"], bufs["outs"]
    cw = kb.sb("cw_s", [128, 31], F32)
    kb.dma("sync", cw[:], cwTd[:, :], writes=[cw])
    cb = kb.sb("cb_s", [128, 1], F32)
    kb.dma("sync", cb[:], cbd[:, :], writes=[cb])
    Dg = kb.sb("Dg", [128, 31, 128], BF16)
    for k in range(31):
        kb.ts("vector", Dg[:, k, :], ident[:, :], cw[:, k:k + 1], None, MUL, None, reads=[ident, cw], writes=[Dg])
    zt_pool = Pool([kb.sb(f"zt{i}", [128, 30 + 512], BF16) for i in range(2)])
    for t in range(SEQ // 512):
        zt = zt_pool.next()
        st = stage.next()
        if t == 0:
            kb.memset("gpsimd", zt[:, 0:30], 0.0, writes=[zt])
            kb.dma("sync", st[:, 30:542], zcTd[:, 0:512], writes=[st])
            kb.copy("gpsimd", zt[:, 30:542], st[:, 30:542], reads=[st], writes=[zt])
        else:
            kb.dma("sync", st[:, 0:542], zcTd[:, t * 512 - 30:t * 512 + 512], writes=[st])
            kb.copy("gpsimd", zt[:, 0:542], st[:, 0:542], reads=[st], writes=[zt])
        ps = pp.next()
        for k in range(31):
            kb.mm(ps, ps[:, :], Dg[:, k, :], zt[:, k:k + 512], k == 0, k == 30, reads=[Dg, zt])
        o = outs.next()
        kb.act(o[:, :], ps[:, :], AF.Identity, reads=[ps, cb], writes=[o], bias=cb[:, 0:1])
        kb.dma("gpsimd", ycTd[:, t * 512:(t + 1) * 512], o[:, :], reads=[o])


def ssm_phase(kb, pp, uTd, ysTd, prm, cst, stage):
    V, G, S = "vector", "gpsimd", "scalar"
    SUB = ALU.subtract

    def ld(name, shape, ap):
        t = kb.sb(name + "_s", shape, F32)
        kb.dma("sync", t[:], ap, writes=[t])
        return t

    LR = ld("LR", [128, 64], prm["LR"][:, :])
    LI = ld("LI", [128, 64], prm["LI"][:, :])
    LDT = ld("LDT", [128, 1], prm["LDT"][:, :])
    BRT = ld("BRT", [128, 64], prm["BRT"][:, :])
    BIT = ld("BIT", [128, 64], prm["BIT"][:, :])
    CR = ld("CR", [128, 64], prm["CR"][:, :])
    CI = ld("CI", [128, 64], prm["CI"][:, :])
    DSK = ld("DSK", [128, 1], prm["DSK"][:, :])
    ident, RM, BM, CMK = cst["ident"], cst["RM"], cst["BM"], cst["CMK"]

    def T(name, w=64):
        return kb.sb("s_" + name, [128, w], F32)

    def M(o, a, b):
        kb.tt(V, o, a, b, MUL, reads=[], writes=[])

    def mul(o, a, b, osl=slice(None), eng=V):
        kb.tt(eng, o[:, osl], a[:, :], b[:, :], MUL, reads=[a, b], writes=[o])

    def add(o, a, b, osl=slice(None), eng=V):
        kb.tt(eng, o[:, osl], a[:, :], b[:, :], ADD, reads=[a, b], writes=[o])

    def sub(o, a, b, osl=slice(None), eng=V):
        kb.tt(eng, o[:, osl], a[:, :], b[:, :], SUB, reads=[a, b], writes=[o])

    lr, dt, m_, ang, sA, cA = T("lr"), T("dt", 1), T("m"), T("ang"), T("sA"), T("cA")
    kb.ts(V, lr[:], LR[:], -1e-4, None, ALU.min, None, reads=[LR], writes=[lr])
    kb.act(dt[:], LDT[:], AF.Exp, reads=[LDT], writes=[dt])
    kb.act(m_[:], lr[:], AF.Exp, reads=[lr, dt], writes=[m_], scale=dt[:, 0:1])
    kb.ts(V, ang[:], LI[:], dt[:, 0:1], None, MUL, None, reads=[LI, dt], writes=[ang])
    scr = dict(kk=T("kk"), ki=kb.sb("s_ki", [128, 64], I32), msk=T("msk"))
    sincos(kb, ang, sA, cA, scr)
    ar, ai, t1, t2, den, nr = T("ar"), T("ai"), T("t1"), T("t2"), T("den"), T("nr")
    mul(ar, m_, cA)
    mul(ai, m_, sA)
    mul(den, lr, lr)
    mul(t1, LI, LI)
    add(den, den, t1)
    kb.recip(den[:], den[:], reads=[den], writes=[den])
    kb.ts(V, nr[:], ar[:], -1.0, None, ADD, None, reads=[ar], writes=[nr])
    wr = [T("wr0"), T("wr1")]
    wi = [T("wi0"), T("wi1")]
    mul(t1, nr, lr)
    mul(t2, ai, LI)
    add(t1, t1, t2)
    mul(wr[0], t1, den)
    mul(t1, ai, lr)
    mul(t2, nr, LI)
    sub(t1, t1, t2)
    mul(wi[0], t1, den)

    LE = kb.sb("LE", [128, 8, 16, 128], BF16)
    LF = kb.sb("LF", [128, 8, 16, 64], BF16)
    BD = kb.sb("BD", [128, 16, 128], BF16)
    X = T("X", 128)
    XT = kb.sb("s_XT", [128, 128], BF16)
    Y = T("Y", 128)
    YT = kb.sb("s_YT", [128, 128], BF16)
    Z = T("Z", 128)
    dD = T("dD", 128)
    tb = T("tb", 128)
    kb.copy(V, Y[:, 0:64], CR[:, :], reads=[CR], writes=[Y])
    kb.ts(V, Y[:, 64:128], CI[:, :], -1.0, None, MUL, None, reads=[CI], writes=[Y])
    ps = pp.next()
    kb.op("tensor", lambda e, ps=ps: e.transpose(ps[:, 0:128], Y[:, :], ident[:, :]), reads=[Y, ident], writes=[ps])
    kb.copy(S, YT[:, :], ps[:, 0:128], reads=[ps], writes=[YT])
    kb.ts(V, dD[:], ident[:, :], DSK[:, 0:1], None, MUL, None, reads=[ident, DSK], writes=[dD])

    for n in range(16):
        cur, nxt = n % 2, (n + 1) % 2
        mul(t1, wr[cur], BRT)
        mul(t2, wi[cur], BIT)
        sub(X, t1, t2, slice(0, 64))
        mul(t1, wr[cur], BIT)
        mul(t2, wi[cur], BRT)
        add(X, t1, t2, slice(64, 128))
        j = 15 - n
        for g in range(8):
            kb.ts(G if g % 2 else V, LE[:, g, j, :], X[:, :], RM[:, g:g + 1], None, MUL, None, reads=[X, RM], writes=[LE])
        ps = pp.next()
        kb.op("tensor", lambda e, ps=ps: e.transpose(ps[:, 0:128], X[:, :], ident[:, :]), reads=[X, ident], writes=[ps])
        kb.copy(S, XT[:, :], ps[:, 0:128], reads=[ps], writes=[XT])
        ps2 = pp.next()
        kb.mm(ps2, ps2[:, 0:128], XT[:, :], YT[:, :], True, True, reads=[XT, YT])
        if n == 0:
            kb.tt(V, tb[:, :], ps2[:, 0:128], BM[:, :], MUL, reads=[ps2, BM], writes=[tb])
            kb.tt(V, BD[:, n, :], tb[:, :], dD[:, :], ADD, reads=[tb, dD], writes=[BD])
        else:
            kb.tt(V, BD[:, n, :], ps2[:, 0:128], BM[:, :], MUL, reads=[ps2, BM], writes=[BD])
        if n < 15:
            mul(t1, wr[cur], ar)
            mul(t2, wi[cur], ai)
            sub(wr[nxt], t1, t2)
            mul(t1, wr[cur], ai)
            mul(t2, wi[cur], ar)
            add(wi[nxt], t1, t2)

    pr = [T("pr0"), T("pr1")]
    pi_ = [T("pi0"), T("pi1")]
    kb.copy(V, pr[1][:], ar[:], reads=[ar], writes=[pr[1]])
    kb.copy(V, pi_[1][:], ai[:], reads=[ai], writes=[pi_[1]])
    for n in range(1, 17):
        cur, nxt = n % 2, (n + 1) % 2
        j = n - 1
        mul(t1, CR, pr[cur])
        mul(t2, CI, pi_[cur])
        sub(Z, t1, t2, slice(0, 64))
        mul(t1, CR, pi_[cur])
        mul(t2, CI, pr[cur])
        add(t1, t1, t2)
        kb.ts(V, Z[:, 64:128], t1[:, :], -1.0, None, MUL, None, reads=[t1], writes=[Z])
        ps = pp.next()
        kb.op("tensor", lambda e, ps=ps: e.transpose(ps[:, 0:128], Z[:, :], ident[:, :]), reads=[Z, ident], writes=[ps])
        for g in range(8):
            h = g // 4
            kb.tt(V, LF[:, g, j, :], ps[:, 64 * h:64 * h + 64], CMK[:, g % 4, :], MUL, reads=[ps, CMK], writes=[LF])
        if n < 16:
            mul(t1, pr[cur], ar)
            mul(t2, pi_[cur], ai)
            sub(pr[nxt], t1, t2)
            mul(t1, pr[cur], ai)
            mul(t2, pi_[cur], ar)
            add(pi_[nxt], t1, t2)
    p16r, p16i = pr[0], pi_[0]
    AL = kb.sb("s_AL", [128, 8], F32)
    B1 = kb.sb("s_B1", [128, 8], F32)
    B2 = kb.sb("s_B2", [128, 8], F32)
    rowt = T("rowt", 128)
    for dst, (sa, sb_) in ((AL, (1.0, 1.0)), (B1, (-1.0, 1.0)), (B2, (1.0, -1.0))):
        src = p16r if dst is AL else p16i
        kb.ts(V, rowt[:, 0:64], src[:, :], sa, None, MUL, None, reads=[src], writes=[rowt])
        kb.ts(V, rowt[:, 64:128], src[:, :], sb_, None, MUL, None, reads=[src], writes=[rowt])
        ps = pp.next()
        kb.op("tensor", lambda e, ps=ps: e.transpose(ps[:, 0:128], rowt[:, :], ident[:, :]), reads=[rowt, ident], writes=[ps])
        kb.copy(V, dst[:, :], ps[:, 0:128].rearrange("p (g r) -> p g r", r=16)[:, :, 0], reads=[ps], writes=[dst])

    QC = 256
    QT = QC * 16
    ub = kb.sb("s_ub", [128, QT], BF16)
    E1 = kb.sb("s_E1", [128, 8, QC], F32)
    E2 = kb.sb("s_E2", [128, 8, QC], F32)
    H1 = kb.sb("s_H1", [128, 8, QC + 1], F32)
    H2 = kb.sb("s_H2", [128, 8, QC + 1], F32)
    H1b = kb.sb("s_H1b", [128, 8, QC], BF16)
    ta = [kb.sb(f"s_ta{i}", [128, 8], F32) for i in range(2)]
    tbb = [kb.sb(f"s_tbb{i}", [128, 8], F32) for i in range(2)]
    yo_pool = Pool([kb.sb(f"s_yo{i}", [128, 512], F32) for i in range(2)])
    kb.memset(V, H1[:, :, 0:1], 0.0, writes=[H1])
    kb.memset(V, H2[:, :, 0:1], 0.0, writes=[H2])
    uv = ub[:, :].rearrange("p (c j) -> p c j", j=16)
    for q in range(SEQ // QT):
        t0 = q * QT
        for i in range(QT // 2048):
            st = stage.next()
            kb.dma("sync", st[:, 0:2048], uTd[:, t0 + i * 2048:t0 + (i + 1) * 2048], writes=[st])
            kb.copy(G, ub[:, i * 2048:(i + 1) * 2048], st[:, 0:2048], reads=[st], writes=[ub])
        if q > 0:
            kb.copy(V, H1[:, :, 0:1], H1[:, :, QC:QC + 1], reads=[H1], writes=[H1])
            kb.copy(V, H2[:, :, 0:1], H2[:, :, QC:QC + 1], reads=[H2], writes=[H2])
        for g in range(8):
            ps1 = pp.next()
            ps2 = pp.next()
            for j in range(16):
                kb.mm(ps1, ps1[:, 0:QC], LE[:, g, j, :], uv[:, :, j], j == 0, j == 15, reads=[LE, ub])
            for j in range(16):
                kb.mm(ps2, ps2[0:64, 0:QC], LE[:, g, j, 64:128], uv[:, :, j], j == 0, j == 15, reads=[LE, ub])
            for j in range(16):
                kb.mm(ps2, ps2[64:128, 0:QC], LE[:, g, j, 0:64], uv[:, :, j], j == 0, j == 15, reads=[LE, ub])
            kb.copy(S, E1[:, g, :], ps1[:, 0:QC], reads=[ps1], writes=[E1])
            kb.copy(S, E2[:, g, :], ps2[:, 0:QC], reads=[ps2], writes=[E2])
        for c in range(QC):
            kb.tt(V, ta[0][:, :], B1[:, :], H2[:, :, c], MUL, reads=[B1, H2], writes=[ta[0]])
            kb.tt(V, ta[1][:, :], B2[:, :], H1[:, :, c], MUL, reads=[B2, H1], writes=[ta[1]])
            kb.tt(V, tbb[0][:, :], AL[:, :], H1[:, :, c], MUL, reads=[AL, H1], writes=[tbb[0]])
            kb.tt(V, tbb[1][:, :], AL[:, :], H2[:, :, c], MUL, reads=[AL, H2], writes=[tbb[1]])
            kb.tt(V, ta[0][:, :], ta[0][:, :], E1[:, :, c], ADD, reads=[ta[0], E1], writes=[ta[0]])
            kb.tt(V, ta[1][:, :], ta[1][:, :], E2[:, :, c], ADD, reads=[ta[1], E2], writes=[ta[1]])
            kb.tt(V, H1[:, :, c + 1], ta[0][:, :], tbb[0][:, :], ADD, reads=[ta[0], tbb[0]], writes=[H1])
            kb.tt(V, H2[:, :, c + 1], ta[1][:, :], tbb[1][:, :], ADD, reads=[ta[1], tbb[1]], writes=[H2])
        kb.copy(G, H1b[:, :, :], H1[:, :, 0:QC], reads=[H1], writes=[H1b])
        for tl in range(QC // 32):
            c0 = tl * 32
            yps = pp.next()
            first = True
            for j in range(16):
                for tau in range(j + 1):
                    kb.mm(yps, yps[:, j * 32:(j + 1) * 32], BD[:, tau, :], uv[:, c0:c0 + 32, j - tau], first, False, reads=[BD, ub])
                    first = False
            for j in range(16):
                for g in range(8):
                    h = g // 4
                    kb.mm(yps, yps[64 * h:64 * h + 64, j * 32:(j + 1) * 32], LF[:, g, j, :], H1b[:, g, c0:c0 + 32], False,
                          (j == 15 and g == 7), reads=[LF, H1b])
            yo = yo_pool.next()
            kb.copy(S, yo[:, :].rearrange("p (c j) -> p j c", j=16), yps[:, :].rearrange("p (j c) -> p j c", c=32), reads=[yps], writes=[yo])
            kb.dma("gpsimd", ysTd[:, t0 + c0 * 16:t0 + c0 * 16 + 512], yo[:, :], reads=[yo])
    return yo_pool


def ssm_consts():
    RM = np.zeros((128, 8), np.float32)
    BM = np.zeros((128, 128), np.float32)
    CMK = np.zeros((128, 4, 64), np.float32)
    for g in range(8):
        RM[16 * g:16 * g + 16, g] = 1.0
        BM[16 * g:16 * g + 16, 16 * g:16 * g + 16] = 1.0
    for q in range(4):
        CMK[:, q, 16 * q:16 * q + 16] = 1.0
    return RM, BM, CMK.reshape(128, 256)


def build_B(do_ssm=True, do_conv=True, do_attn=True):
    nc = bass.Bass("TRN2", target_bir_lowering=False)
    kb = KB(nc)
    qTd = kb.dram("qT", [192, SEQ], BF16, "ExternalInput")
    kTd = kb.dram("kT", [192, SEQ], BF16, "ExternalInput")
    Vd = kb.dram("V", [SEQ, 128], BF16, "ExternalInput")
    uTd = kb.dram("uT", [128, SEQ], F32, "ExternalInput")
    zcTd = kb.dram("zcT", [128, SEQ], F32, "ExternalInput")
    prm = {n: kb.dram(n, [128, 64], F32, "ExternalInput") for n in ("LR", "LI", "BRT", "BIT", "CR", "CI")}
    prm["LDT"] = kb.dram("LDT", [128, 1], F32, "ExternalInput")
    prm["DSK"] = kb.dram("DSK", [128, 1], F32, "ExternalInput")
    cwTd = kb.dram("cwT", [128, 31], F32, "ExternalInput")
    cbd = kb.dram("cb", [128, 1], F32, "ExternalInput")
    identd = kb.dram("ident", [128, 128], F32, "ExternalInput")
    RMd = kb.dram("RM", [128, 8], F32, "ExternalInput")
    BMd = kb.dram("BM", [128, 128], F32, "ExternalInput")
    CMKd = kb.dram("CMK", [128, 256], F32, "ExternalInput")
    OTd = kb.dram("OT", [130, SEQ], F32, "ExternalOutput")
    ysTd = kb.dram("ysT", [128, SEQ], F32, "ExternalOutput")
    ycTd = kb.dram("ycT", [128, SEQ], F32, "ExternalOutput")

    pp = Pool([kb.ps(f"ps{i}") for i in range(8)])
    stage = Pool([kb.sb(f"stg{i}", [128, 2048], F32) for i in range(2)])
    ident = kb.sb("ident_s", [128, 128], F32)
    kb.dma("sync", ident[:], identd[:, :], writes=[ident])
    outs = Pool([kb.sb(f"outs{i}", [128, 512], F32) for i in range(3)])
    allouts = list(outs.bufs)
    if do_conv:
        conv_phase(kb, pp, zcTd, cwTd, cbd, identd, ycTd, dict(stage=stage, ident=ident, outs=outs))
    if do_ssm:
        RM = kb.sb("RM_s", [128, 8], F32)
        kb.dma("sync", RM[:], RMd[:, :], writes=[RM])
        BM = kb.sb("BM_s", [128, 128], F32)
        kb.dma("sync", BM[:], BMd[:, :], writes=[BM])
        CMK = kb.sb("CMK_s", [128, 4, 64], F32)
        kb.dma("sync", CMK[:], CMKd.rearrange("p (q f) -> p q f", q=4), writes=[CMK])
        yo_pool = ssm_phase(kb, pp, uTd, ysTd, prm, dict(ident=ident, RM=RM, BM=BM, CMK=CMK), stage)
        allouts += yo_pool.bufs
    if do_attn:
        bufs = dict(kT=kb.sb("kT_s", [128, SEQ], BF16), qT=Pool([kb.sb(f"qT_s{i}", [128, 512], BF16) for i in range(3)]), V=kb.sb("Vs", [128, 128, 65], BF16),
                    pts=Pool([kb.sb(f"pt{i}", [128, 512], BF16) for i in range(4)]), outs=outs)
        pp_s = Pool(pp.bufs[0:5])
        pp_o = Pool(pp.bufs[5:8])
        for hl in range(2):
            attention_head(kb, pp_s, pp_o, qTd, kTd, Vd, OTd, hl, bufs)
    kb.final_wait("gpsimd", allouts)
    return kb.build()


def prep_B(inp, l, kblk):
    gs = slice(8 * kblk, 8 * kblk + 8)
    rep = lambda a: np.ascontiguousarray(np.repeat(a, 16, axis=0).astype(np.float32))
    RM, BM, CMK = ssm_consts()
    d = dict(
        LR=rep(inp["ssm_lam_re"][l, gs]), LI=rep(inp["ssm_lam_im"][l, gs]),
        LDT=rep(inp["ssm_log_dt"][l, gs].reshape(8, 1)),
        BRT=np.ascontiguousarray(inp["ssm_b_re"][l, gs].transpose(0, 2, 1).reshape(128, 64)),
        BIT=np.ascontiguousarray(inp["ssm_b_im"][l, gs].transpose(0, 2, 1).reshape(128, 64)),
        CR=np.ascontiguousarray(inp["ssm_c_re"][l, gs].reshape(128, 64)),
        CI=np.ascontiguousarray(inp["ssm_c_im"][l, gs].reshape(128, 64)),
        DSK=np.ascontiguousarray(inp["ssm_d"][l, 128 * kblk:128 * kblk + 128].reshape(128, 1)),
        cwT=np.ascontiguousarray(inp["conv_w"][l][:, 128 * kblk:128 * kblk + 128].T),
        cb=np.ascontiguousarray(inp["conv_b"][l, 128 * kblk:128 * kblk + 128].reshape(128, 1)),
        ident=np.eye(128, dtype=np.float32), RM=RM, BM=BM, CMK=CMK)
    return d


TC = 256
LN_EPS = 1e-5
GELU_C = 0.7978845608028654


def build_C1():
    nc = bass.Bass("TRN2", target_bir_lowering=False)
    kb = KB(nc)
    xT = kb.dram("xT", [D, NTOK], F32, "ExternalInput")
    OTd = kb.dram("OT", [8 * 65, NTOK], F32, "ExternalInput")
    ysTd = kb.dram("ysT", [512, NTOK], F32, "ExternalInput")
    ycTd = kb.dram("ycT", [512, NTOK], F32, "ExternalInput")
    gmix = kb.dram("gmix", [128, 8], F32, "ExternalInput")
    WG = kb.dram("WG", [D, 3072], F32, "ExternalInput")
    WOA = kb.dram("WOA", [512, D], F32, "ExternalInput")
    WOS = kb.dram("WOS", [512, D], F32, "ExternalInput")
    WOC = kb.dram("WOC", [512, D], F32, "ExternalInput")
    WOUT = kb.dram("WOUT", [D, D], F32, "ExternalInput")
    WGLU = kb.dram("WGLU", [512, 512], F32, "ExternalInput")
    bglu = kb.dram("bglu", [128, 4], F32, "ExternalInput")
    lng = kb.dram("lng", [128, 4], F32, "ExternalInput")
    lnb = kb.dram("lnb", [128, 4], F32, "ExternalInput")
    x1T = kb.dram("x1T", [D, NTOK], F32, "ExternalOutput")

    make_consts(kb)
    eps5 = kb.sb("eps5", [128, 1], F32)
    kb.memset("vector", eps5[:], LN_EPS, writes=[eps5])
    pp = Pool([kb.ps(f"ps{i}") for i in range(8)])
    stage = Pool([kb.sb(f"stg{i}", [128, 1024], F32) for i in range(2)])
    V, G, S = "vector", "gpsimd", "scalar"

    wg = kb.sb("wg", [128, 8, 3072], BF16)
    load_cast(kb, wg, [wg[:, k, c * 1024:(c + 1) * 1024] for k in range(8) for c in range(3)],
              [WG[k * 128:(k + 1) * 128, c * 1024:(c + 1) * 1024] for k in range(8) for c in range(3)], stage)
    woa = kb.sb("woa", [128, 8, D], BF16)
    load_cast(kb, woa, [woa[0:64, hh, :] for hh in range(8)], [WOA[hh * 64:(hh + 1) * 64, :] for hh in range(8)], stage)
    wos = kb.sb("wos", [128, 4, D], BF16)
    load_cast(kb, wos, [wos[:, k, :] for k in range(4)], [WOS[k * 128:(k + 1) * 128, :] for k in range(4)], stage)
    woc = kb.sb("woc", [128, 4, D], BF16)
    load_cast(kb, woc, [woc[:, k, :] for k in range(4)], [WOC[k * 128:(k + 1) * 128, :] for k in range(4)], stage)
    wout = kb.sb("wout", [128, 8, D], BF16)
    load_cast(kb, wout, [wout[:, k, :] for k in range(8)], [WOUT[k * 128:(k + 1) * 128, :] for k in range(8)], stage)
    wglu = kb.sb("wglu", [128, 4, 512], BF16)
    load_cast(kb, wglu, [wglu[:, k, :] for k in range(4)], [WGLU[k * 128:(k + 1) * 128, :] for k in range(4)], stage)

    def small(name, ap, w):
        t = kb.sb(name, [128, w], F32)
        kb.dma("sync", t[:], ap, writes=[t])
        return t

    gm = small("gm", gmix[:, :], 8)
    bg = small("bg", bglu[:, :], 4)
    lg_ = small("lg", lng[:, :], 4)
    lb_ = small("lb", lnb[:, :], 4)

    xs = kb.sb("xs", [128, 8, TC], F32)
    sq = kb.sb("sq", [128, 8, TC], BF16)
    h = kb.sb("h", [128, 8, TC], BF16)
    rstd = kb.sb("rstd", [128, TC], F32)
    tmp = kb.sb("tmp", [128, TC], F32)
    o_pool = Pool([kb.sb(f"o{i}", [128, TC], F32) for i in range(2)])
    d_pool = Pool([kb.sb(f"dn{i}", [128, TC], F32) for i in range(2)])
    attn = kb.sb("attn", [128, 8, TC], BF16)
    ys = kb.sb("ys", [128, 4, TC], F32)
    tg = kb.sb("tg", [128, 4, TC], F32)
    sbf = kb.sb("sbf", [128, 4, TC], BF16)
    s2 = kb.sb("s2", [128, 4, TC], BF16)
    yc = kb.sb("yc", [128, 4, TC], F32)
    ycb = kb.sb("ycb", [128, 4, TC], BF16)
    cbf = kb.sb("cbf", [128, 4, TC], BF16)
    nmean = kb.sb("nmean", [128, TC], F32)
    merged = kb.sb("merged", [128, 8, TC], BF16)
    gs_ = [kb.sb(f"gsig{i}", [128, TC], F32) for i in range(3)]
    m0 = kb.sb("m0", [128, TC], F32)
    m1 = kb.sb("m1", [128, TC], F32)
    sgl = kb.sb("sgl", [128, TC], F32)

    xv = xT.rearrange("(k p) t -> p k t", p=128)
    x1v = x1T.rearrange("(k p) t -> p k t", p=128)
    ysv = ysTd.rearrange("(k p) t -> p k t", p=128)
    ycv = ycTd.rearrange("(k p) t -> p k t", p=128)
    for it in range(NTOK // TC):
        c0 = it * TC
        cs = slice(c0, c0 + TC)
        kb.dma("sync", xs[:], xv[:, :, cs], writes=[xs])
        kb.dma("sync", ys[:], ysv[:, :, cs], writes=[ys])
        kb.dma("sync", yc[:], ycv[:, :, cs], writes=[yc])
        rms_stats(kb, pp, xs, 8, TC, sq, rstd, tmp, 1.0 / D)
        for k in range(8):
            kb.stt(h[:, k, :], xs[:, k, :], gm[:, k:k + 1], rstd[:], MUL, MUL, reads=[xs, gm, rstd], writes=[h])
        for hh in range(8):
            o = o_pool.next()
            dn = d_pool.next()
            kb.dma("sync", o[0:64, :], OTd[hh * 65:hh * 65 + 64, cs], writes=[o])
            kb.dma("sync", dn[0:64, :], OTd[hh * 65 + 64:hh * 65 + 65, cs].partition_broadcast(64), writes=[dn])
            kb.recip(dn[0:64, :], dn[0:64, :], reads=[dn], writes=[dn])
            kb.tt(V, attn[0:64, hh, :], o[0:64, :], dn[0:64, :], MUL, reads=[o, dn], writes=[attn])
        kb.tt(V, tg[:], ys[:], ys[:], MUL, reads=[ys], writes=[tg])
        kb.ts(V, tg[:], tg[:], 0.044715, 1.0, MUL, ADD, reads=[tg], writes=[tg])
        kb.tt(V, tg[:], tg[:], ys[:], MUL, reads=[tg, ys], writes=[tg])
        kb.act(tg[:], tg[:], AF.Sigmoid, reads=[tg], writes=[tg], scale=2.0 * GELU_C)
        kb.tt(V, ys[:], ys[:], tg[:], MUL, reads=[ys, tg], writes=[ys])
        kb.copy(G, sbf[:], ys[:], reads=[ys], writes=[sbf])
        for m in range(4):
            ps = pp.next()
            for k in range(4):
                kb.mm(ps, ps[:, 0:TC], wglu[:, k, m * 128:(m + 1) * 128], sbf[:, k, :], k == 0, k == 3, reads=[wglu, sbf])
            kb.act(sgl[:], ps[:, 0:TC], AF.Sigmoid, reads=[ps, bg], writes=[sgl], bias=bg[:, m:m + 1])
            kb.tt(V, s2[:, m, :], ys[:, m, :], sgl[:], MUL, reads=[ys, sgl], writes=[s2])
        kb.copy(G, ycb[:], yc[:], reads=[yc], writes=[ycb])
        ps = pp.next()
        for k in range(4):
            kb.mm(ps, ps[:, 0:TC], kb.ones_bf[:], ycb[:, k, :], k == 0, k == 3, reads=[kb.ones_bf, ycb])
        kb.act(nmean[:], ps[:, 0:TC], AF.Copy, reads=[ps], writes=[nmean], scale=-1.0 / 512)
        for k in range(4):
            kb.tt(V, yc[:, k, :], yc[:, k, :], nmean[:], ADD, reads=[yc, nmean], writes=[yc])
        kb.act(sq[:, 0:4, :], yc[:, :, :], AF.Square, reads=[yc], writes=[sq])
        ps = pp.next()
        for k in range(4):
            kb.mm(ps, ps[:, 0:TC], kb.ones_bf[:], sq[:, k, :], k == 0, k == 3, reads=[kb.ones_bf, sq])
        kb.act(tmp[:], ps[:, 0:TC], AF.Sqrt, reads=[ps, eps5], writes=[tmp], bias=eps5[:, 0:1], scale=1.0 / 512)
        kb.recip(rstd[:], tmp[:], reads=[tmp], writes=[rstd])
        for k in range(4):
            kb.stt(yc[:, k, :], yc[:, k, :], lg_[:, k:k + 1], rstd[:], MUL, MUL, reads=[yc, lg_, rstd], writes=[yc])
            kb.act(cbf[:, k, :], yc[:, k, :], AF.Silu, reads=[yc, lb_], writes=[cbf], bias=lb_[:, k:k + 1])
        for m in range(8):
            ms = slice(m * 128, (m + 1) * 128)
            pa, ps_, pc = pp.next(), pp.next(), pp.next()
            for hh in range(8):
                kb.mm(pa, pa[:, 0:TC], woa[0:64, hh, ms], attn[0:64, hh, :], hh == 0, hh == 7, reads=[woa, attn])
            for k in range(4):
                kb.mm(ps_, ps_[:, 0:TC], wos[:, k, ms], s2[:, k, :], k == 0, k == 3, reads=[wos, s2])
            for k in range(4):
                kb.mm(pc, pc[:, 0:TC], woc[:, k, ms], cbf[:, k, :], k == 0, k == 3, reads=[woc, cbf])
            pg = [pp.next() for _ in range(3)]
            for i in range(3):
                for k in range(8):
                    kb.mm(pg[i], pg[i][:, 0:TC], wg[:, k, i * 1024 + m * 128:i * 1024 + (m + 1) * 128], h[:, k, :], k == 0, k == 7, reads=[wg, h])
                kb.act(gs_[i][:], pg[i][:, 0:TC], AF.Sigmoid, reads=[pg[i]], writes=[gs_[i]])
            kb.tt(V, m0[:], pa[:, 0:TC], gs_[0][:], MUL, reads=[pa, gs_[0]], writes=[m0])
            kb.tt(V, m1[:], ps_[:, 0:TC], gs_[1][:], MUL, reads=[ps_, gs_[1]], writes=[m1])
            kb.tt(G, m0[:], m0[:], m1[:], ADD, reads=[m0, m1], writes=[m0])
            kb.tt(V, m1[:], pc[:, 0:TC], gs_[2][:], MUL, reads=[pc, gs_[2]], writes=[m1])
            kb.tt(G, merged[:, m, :], m0[:], m1[:], ADD, reads=[m0, m1], writes=[merged])
        for m2 in range(8):
            ps = pp.next()
            for m in range(8):
                kb.mm(ps, ps[:, 0:TC], wout[:, m, m2 * 128:(m2 + 1) * 128], merged[:, m, :], m == 0, m == 7, reads=[wout, merged])
            kb.tt(V, xs[:, m2, :], xs[:, m2, :], ps[:, 0:TC], ADD, reads=[xs, ps], writes=[xs])
        kb.dma("gpsimd", x1v[:, :, cs], xs[:], reads=[xs])
    kb.final_wait("gpsimd", [xs])
    return kb.build()


def prep_C1(inp, l):
    return dict(gmix=_col(inp["norm_mix"][l], 8), WG=np.ascontiguousarray(inp["w_in"][l][:, 1952:5024]),
                WOA=np.ascontiguousarray(inp["w_o_attn"][l]), WOS=np.ascontiguousarray(inp["w_o_ssm"][l]),
                WOC=np.ascontiguousarray(inp["w_o_conv"][l]), WOUT=np.ascontiguousarray(inp["w_out"][l]),
                WGLU=np.ascontiguousarray(inp["ssm_w_glu"][l]), bglu=_col(inp["ssm_b_glu"][l], 4),
                lng=_col(inp["conv_ln_g"][l], 4), lnb=_col(inp["conv_ln_b"][l], 4))


FG = 256
HALF = 2048


def build_C2(E, FF, moe, final):
    nc = bass.Bass("TRN2", target_bir_lowering=False)
    kb = KB(nc)
    x1T = kb.dram("x1T", [D, NTOK], F32, "ExternalInput")
    gffn = kb.dram("gffn", [128, 8], F32, "ExternalInput")
    W1 = kb.dram("W1", [E * D, FF], F32, "ExternalInput")
    W3 = kb.dram("W3", [E * D, FF], F32, "ExternalInput")
    W2 = kb.dram("W2", [E * FF, D], F32, "ExternalInput")
    if moe:
        WR = kb.dram("WR", [D, 8], F32, "ExternalInput")
        SELd = kb.dram("SEL", [8, 8 * 128], F32, "ExternalInput")
        identd = kb.dram("ident", [128, 128], F32, "ExternalInput")
    if final:
        gfin = kb.dram("gfin", [128, 8], F32, "ExternalInput")
    outT = kb.dram("outT", [D, NTOK], F32, "ExternalOutput")

    make_consts(kb)
    V, G, S = "vector", "gpsimd", "scalar"
    pp = Pool([kb.ps(f"ps{i}") for i in range(8)])
    stage = Pool([kb.sb(f"stg{i}", [128, 8, FG], F32) for i in range(2)])
    NFC = FG // 128
    w1p = Pool([kb.sb(f"w1g{i}", [128, 8, FG], BF16) for i in range(2)])
    w3p = Pool([kb.sb(f"w3g{i}", [128, 8, FG], BF16) for i in range(2)])
    w2p = Pool([kb.sb(f"w2g{i}", [128, NFC, D], BF16) for i in range(2)])
    gm = kb.sb("gm", [128, 8], F32)
    kb.dma("sync", gm[:], gffn[:, :], writes=[gm])
    if final:
        gf = kb.sb("gf", [128, 8], F32)
        kb.dma("sync", gf[:], gfin[:, :], writes=[gf])
    acc = kb.sb("acc", [128, 8, HALF], F32)
    h2 = kb.sb("h2", [128, 8, HALF], BF16)
    h2f = kb.sb("h2f", [128, 8, TT], F32)
    sq = kb.sb("sq", [128, 8, TT], BF16)
    rstd = kb.sb("rstd", [128, TT], F32)
    tmp = kb.sb("tmp", [128, TT], F32)
    sa = kb.sb("sa", [128, TT], F32)
    pr = kb.sb("pr", [128, TT], F32)
    pb = kb.sb("pb", [128, NFC, TT], BF16)
    if moe:
        wr = kb.sb("wr", [128, 8, 8], F32)
        kb.dma("sync", wr[:], WR.rearrange("(k p) e -> p k e", p=128), writes=[wr])
        SEL = kb.sb("SEL_s", [8, 8 * 128], F32)
        kb.dma("sync", SEL[:], SELd[:, :], writes=[SEL])
        ident = kb.sb("ident_s", [128, 128], F32)
        kb.dma("sync", ident[:], identd[:, :], writes=[ident])
        lg = kb.sb("lg", [8, TT], F32)
        LT = kb.sb("LT", [128, 4, 8], F32)
        mx8 = kb.sb("mx8", [128, 4, 8], F32)
        dd = kb.sb("dd", [128, 4], F32)
        g1 = kb.sb("g1", [128, 4], F32)
        g2 = kb.sb("g2", [128, 4], F32)
        Wt = kb.sb("Wt", [128, 4, 8], F32)
        Wt2 = kb.sb("Wt2", [128, 4, 8], F32)
        WT = kb.sb("WT", [8, HALF], F32)
        GBe = kb.sb("GBe", [128, HALF], F32)

    xv = x1T.rearrange("(k p) t -> p k t", p=128)
    ov = outT.rearrange("(k p) t -> p k t", p=128)
    W1v = W1.rearrange("(e k p) f -> e p k f", p=128, k=8)
    W3v = W3.rearrange("(e k p) f -> e p k f", p=128, k=8)
    W2v = W2.rearrange("(e c p) n -> e p c n", p=128, c=FF // 128)
    for hf in range(NTOK // HALF):
        h0 = hf * HALF
        for k in range(8):
            kb.dma("sync", acc[:, k, :], xv[:, k, h0:h0 + HALF], writes=[acc])
        for tl in range(HALF // TT):
            ts_ = slice(tl * TT, (tl + 1) * TT)
            kb.act(sq[:], acc[:, :, ts_], AF.Square, reads=[acc], writes=[sq])
            ps = pp.next()
            for k in range(8):
                kb.mm(ps, ps[:, :], kb.ones_bf[:], sq[:, k, :], k == 0, k == 7, reads=[kb.ones_bf, sq])
            kb.act(tmp[:], ps[:, :], AF.Sqrt, reads=[ps, kb.eps_t], writes=[tmp], bias=kb.eps_t[:, 0:1], scale=1.0 / D)
            kb.recip(rstd[:], tmp[:], reads=[tmp], writes=[rstd])
            for k in range(8):
                kb.stt(h2f[:, k, :], acc[:, k, ts_], gm[:, k:k + 1], rstd[:], MUL, MUL, reads=[acc, gm, rstd], writes=[h2f])
            kb.copy(G, h2[:, :, ts_], h2f[:, :, :], reads=[h2f], writes=[h2])
            if moe:
                ps = pp.next()
                for k in range(8):
                    kb.mm(ps, ps[0:8, :], wr[:, k, :], h2f[:, k, :], k == 0, k == 7, reads=[wr, h2f])
                kb.copy(S, lg[0:8, :], ps[0:8, :], reads=[ps], writes=[lg])
                ps = pp.next()
                for sub in range(4):
                    kb.op("tensor", lambda e, ps=ps, sub=sub: e.transpose(ps[:, sub * 8:(sub + 1) * 8], lg[0:8, sub * 128:(sub + 1) * 128], ident[0:8, 0:8]),
                          reads=[lg, ident], writes=[ps])
                kb.copy(V, LT[:, :, :], ps[:, 0:32].rearrange("p (s e) -> p s e", e=8), reads=[ps], writes=[LT])
                for sub in range(4):
                    kb.op("vector", lambda e, sub=sub: e.max(mx8[:, sub, :], LT[:, sub, :]), reads=[LT], writes=[mx8])
                kb.tt(V, dd[:, :], mx8[:, :, 1], mx8[:, :, 0], ALU.subtract, reads=[mx8], writes=[dd])
                kb.act(dd[:, :], dd[:, :], AF.Exp, reads=[dd], writes=[dd])
                kb.ts(V, g1[:, :], dd[:, :], 1.0, None, ADD, None, reads=[dd], writes=[g1])
                kb.recip(g1[:, :], g1[:, :], reads=[g1], writes=[g1])
                kb.tt(V, g2[:, :], dd[:, :], g1[:, :], MUL, reads=[dd, g1], writes=[g2])
                for sub in range(4):
                    kb.ts(V, Wt[:, sub, :], LT[:, sub, :], mx8[:, sub, 0:1], g1[:, sub:sub + 1], ALU.is_equal, MUL, reads=[LT, mx8, g1], writes=[Wt])
                    kb.ts(V, Wt2[:, sub, :], LT[:, sub, :], mx8[:, sub, 1:2], g2[:, sub:sub + 1], ALU.is_equal, MUL, reads=[LT, mx8, g2], writes=[Wt2])
                kb.tt(V, Wt[:, :, :], Wt[:, :, :], Wt2[:, :, :], ADD, reads=[Wt, Wt2], writes=[Wt])
                ps = pp.next()
                for sub in range(4):
                    kb.op("tensor", lambda e, ps=ps, sub=sub: e.transpose(ps[0:8, sub * 128:(sub + 1) * 128], Wt[:, sub, :], ident[:, :]),
                          reads=[Wt, ident], writes=[ps])
                kb.copy(S, WT[0:8, ts_], ps[0:8, :], reads=[ps], writes=[WT])
        for e_ in range(E):
            if moe:
                for tl in range(HALF // TT):
                    ts_ = slice(tl * TT, (tl + 1) * TT)
                    ps = pp.next()
                    kb.mm(ps, ps[:, :], SEL[0:8, e_ * 128:(e_ + 1) * 128], WT[0:8, ts_], True, True, reads=[SEL, WT])
                    kb.copy(S, GBe[:, ts_], ps[:, :], reads=[ps], writes=[GBe])
            for fg in range(FF // FG):
                fs = slice(fg * FG, (fg + 1) * FG)
                w1g, w3g, w2g = w1p.next(), w3p.next(), w2p.next()
                st = stage.next()
                kb.dma("sync", st[:, :, :], W1v[e_, :, :, fs], writes=[st])
                kb.copy(S, w1g[:, :, :], st[:, :, :], reads=[st], writes=[w1g])
                st = stage.next()
                kb.dma("sync", st[:, :, :], W3v[e_, :, :, fs], writes=[st])
                kb.copy(G, w3g[:, :, :], st[:, :, :], reads=[st], writes=[w3g])
                st = stage.next()
                stv = st[:, :, :].rearrange("p k f -> p (k f)").rearrange("p (c n) -> p c n", c=NFC)
                kb.dma("sync", stv, W2v[e_, :, fg * NFC:(fg + 1) * NFC, :], writes=[st])
                kb.copy(G if fg % 2 else S, w2g[:, :, :], stv, reads=[st], writes=[w2g])
                for tl in range(HALF // TT):
                    ts_ = slice(tl * TT, (tl + 1) * TT)
                    for fc in range(NFC):
                        pa, pb_ = pp.next(), pp.next()
                        for k in range(8):
                            kb.mm(pa, pa[:, :], w1g[:, k, fc * 128:(fc + 1) * 128], h2[:, k, ts_], k == 0, k == 7, reads=[w1g, h2])
                        for k in range(8):
                            kb.mm(pb_, pb_[:, :], w3g[:, k, fc * 128:(fc + 1) * 128], h2[:, k, ts_], k == 0, k == 7, reads=[w3g, h2])
                        kb.act(sa[:], pa[:, :], AF.Silu, reads=[pa], writes=[sa])
                        if moe:
                            kb.tt(V, pr[:], sa[:], pb_[:, :], MUL, reads=[sa, pb_], writes=[pr])
                            kb.tt(G, pb[:, fc, :], pr[:], GBe[:, ts_], MUL, reads=[pr, GBe], writes=[pb])
                        else:
                            kb.tt(V, pb[:, fc, :], sa[:], pb_[:, :], MUL, reads=[sa, pb_], writes=[pb])
                    for m in range(8):
                        po = pp.next()
                        for fc in range(NFC):
                            kb.mm(po, po[:, :], w2g[:, fc, m * 128:(m + 1) * 128], pb[:, fc, :], fc == 0, fc == NFC - 1, reads=[w2g, pb])
                        kb.tt(V, acc[:, m, ts_], acc[:, m, ts_], po[:, :], ADD, reads=[acc, po], writes=[acc])
        if final:
            for tl in range(HALF // TT):
                ts_ = slice(tl * TT, (tl + 1) * TT)
                kb.act(sq[:], acc[:, :, ts_], AF.Square, reads=[acc], writes=[sq])
                ps = pp.next()
                for k in range(8):
                    kb.mm(ps, ps[:, :], kb.ones_bf[:], sq[:, k, :], k == 0, k == 7, reads=[kb.ones_bf, sq])
                kb.act(tmp[:], ps[:, :], AF.Sqrt, reads=[ps, kb.eps_t], writes=[tmp], bias=kb.eps_t[:, 0:1], scale=1.0 / D)
                kb.recip(rstd[:], tmp[:], reads=[tmp], writes=[rstd])
                for k in range(8):
                    kb.stt(acc[:, k, ts_], acc[:, k, ts_], gf[:, k:k + 1], rstd[:], MUL, MUL, reads=[acc, gf, rstd], writes=[acc])
        for k in range(8):
            kb.dma("gpsimd", ov[:, k, h0:h0 + HALF], acc[:, k, :], reads=[acc])
    kb.final_wait("gpsimd", [acc])
    return kb.build()


def prep_C2(inp, l):
    i = l // 2
    if l % 2 == 0:
        d = dict(gffn=_col(inp["norm_ffn"][l], 8), W1=np.ascontiguousarray(inp["ffn_w1"][i]), W3=np.ascontiguousarray(inp["ffn_w3"][i]),
                 W2=np.ascontiguousarray(inp["ffn_w2"][i]))
    else:
        sel = np.zeros((8, 8, 128), np.float32)
        for e in range(8):
            sel[e, e, :] = 1.0
        d = dict(gffn=_col(inp["norm_ffn"][l], 8), W1=np.ascontiguousarray(inp["moe_w1"][i].reshape(8 * D, 3584)),
                 W3=np.ascontiguousarray(inp["moe_w3"][i].reshape(8 * D, 3584)), W2=np.ascontiguousarray(inp["moe_w2"][i].reshape(8 * 3584, D)),
                 WR=np.ascontiguousarray(inp["moe_router"][i]), SEL=sel.reshape(8, 1024), ident=np.eye(128, dtype=np.float32))
    return d


def _run(nc, in_maps):
    res = run_bass_kernel_spmd(nc, in_maps, core_ids=list(range(8)))
    return res.results


def kernel(**inputs):
    inp = {k: np.asarray(v) for k, v in inputs.items()}
    x = inp["x"]
    pos = inp["positions"].astype(np.int32)
    xT = [np.ascontiguousarray(x[c // 4, (c % 4) * NTOK:(c % 4 + 1) * NTOK, :].T) for c in range(8)]
    for l in range(2):
        common = prep_A(inp, l)
        maps = []
        for c in range(8):
            b, qd = c // 4, c % 4
            m = dict(common)
            m["xT"] = xT[c]
            m["pos"] = np.ascontiguousarray(pos[b:b + 1, qd * NTOK:(qd + 1) * NTOK])
            maps.append(m)
        rA = _run(build_A(), maps)
        maps = []
        for c in range(8):
            b, kblk = c // 4, c % 4
            cat = lambda k, ax: np.concatenate([np.asarray(rA[4 * b + qd][k]) for qd in range(4)], axis=ax)
            m = prep_B(inp, l, kblk)
            m.update(qT=np.ascontiguousarray(cat("qT", 1)[192 * kblk:192 * kblk + 192]),
                     kT=np.ascontiguousarray(cat("kT", 1)[192 * kblk:192 * kblk + 192]),
                     V=np.ascontiguousarray(cat("V", 0)[:, 128 * kblk:128 * kblk + 128]),
                     uT=np.ascontiguousarray(cat("uT", 1)[128 * kblk:128 * kblk + 128]),
                     zcT=np.ascontiguousarray(cat("zcT", 1)[128 * kblk:128 * kblk + 128]))
            maps.append(m)
        del rA
        rB = _run(build_B(), maps)
        common = prep_C1(inp, l)
        maps = []
        for c in range(8):
            b, qd = c // 4, c % 4
            ts = slice(qd * NTOK, (qd + 1) * NTOK)
            OT = np.concatenate([np.asarray(rB[4 * b + k]["OT"]).reshape(2, 65, SEQ)[:, :, ts] for k in range(4)], axis=0).reshape(8 * 65, NTOK)
            ysT = np.concatenate([np.asarray(rB[4 * b + k]["ysT"])[:, ts] for k in range(4)], axis=0)
            ycT = np.concatenate([np.asarray(rB[4 * b + k]["ycT"])[:, ts] for k in range(4)], axis=0)
            m = dict(common)
            m.update(xT=xT[c], OT=np.ascontiguousarray(OT), ysT=np.ascontiguousarray(ysT), ycT=np.ascontiguousarray(ycT))
            maps.append(m)
        del rB
        rC1 = _run(build_C1(), maps)
        common = prep_C2(inp, l)
        maps = []
        for c in range(8):
            m = dict(common)
            m["x1T"] = np.asarray(rC1[c]["x1T"])
            if l == 1:
                m["gfin"] = _col(inp["norm_final"], 8)
            maps.append(m)
        del rC1
        nc = build_C2(1, 2816, False, False) if l % 2 == 0 else build_C2(8, 3584, True, l == 1)
        rC2 = _run(nc, maps)
        xT = [np.asarray(rC2[c]["outT"]) for c in range(8)]
    out = np.empty((2, SEQ, D), np.float32)
    for c in range(8):
        out[c // 4, (c % 4) * NTOK:(c % 4 + 1) * NTOK, :] = xT[c].T
    return out
```
